# Optimizing a Trainium2 kernel written in Bass

```python
import math
import jax, jax.numpy as jnp
from jax import lax
import numpy as np

D_MODEL = 4096
BATCH = 4
SEQ = 2048
DEPTH = 1
DEC_BATCH = 128
DEC_SEQ = 4
PAST_LEN = 16384
PAGE_SIZE = 128

N_MEM = 256
CONV_CH = D_MODEL // 4
CONV_WIDTH = 31
CONV_STATE = CONV_WIDTH - 1
RET_HEADS = 8
RET_DK = D_MODEL // 16
RET_DV = D_MODEL // 16
RET_CHUNK = 128
MEM_HEADS = 4
MEM_DH = D_MODEL // 16
N_BRANCH = 3
PEER_HEADS = 8
PEER_NKEYS = 128
PEER_N = PEER_NKEYS * PEER_NKEYS
PEER_DQ = 256
PEER_TOPK = 16
PEER_BLOCK = 32
ROPE_BASE = 10000.0
EPS = 1e-6

IN_SIZES = (2 * CONV_CH, RET_HEADS * RET_DK, RET_HEADS * RET_DK, RET_HEADS * RET_DV,
            RET_HEADS * RET_DV, MEM_HEADS * MEM_DH, N_BRANCH * D_MODEL)
N_IN = sum(IN_SIZES)

kernel_name = 'gated_conv_retention_memory_peer_step'


def _split_cols(z):
    parts = []
    start = 0
    for n in IN_SIZES:
        parts.append(z[..., start:start + n])
        start += n
    return parts


def rmsnorm(x, g):
    xf = x.astype(jnp.float32)
    y = xf * lax.rsqrt(jnp.mean(xf * xf, axis=-1, keepdims=True) + EPS)
    return (y * g.astype(jnp.float32)).astype(x.dtype)


def layernorm(x, g, b):
    xf = x.astype(jnp.float32)
    mu = jnp.mean(xf, axis=-1, keepdims=True)
    xc = xf - mu
    y = xc * lax.rsqrt(jnp.mean(xc * xc, axis=-1, keepdims=True) + EPS)
    return (y * g.astype(jnp.float32) + b.astype(jnp.float32)).astype(x.dtype)


def rotary(x, pos):
    half = x.shape[-1] // 2
    inv = ROPE_BASE ** (-jnp.arange(half, dtype=jnp.float32) / half)
    ang = pos[:, None] * inv[None, :]
    cos = jnp.cos(ang)[None, :, None, :]
    sin = jnp.sin(ang)[None, :, None, :]
    xf = x.astype(jnp.float32)
    x1, x2 = xf[..., :half], xf[..., half:]
    return jnp.concatenate([x1 * cos - x2 * sin, x1 * sin + x2 * cos], axis=-1)


def ret_log_gamma():
    return jnp.log1p(-jnp.exp2(-5.0 - jnp.arange(RET_HEADS, dtype=jnp.float32)))


def retention(q, k, v, s0):
    B, T = q.shape[0], q.shape[1]
    C = math.gcd(T, RET_CHUNK)
    n = T // C
    lg = ret_log_gamma()
    idx = jnp.arange(C, dtype=jnp.float32)
    diff = idx[:, None] - idx[None, :]
    decay_in = jnp.where(diff[None] >= 0.0,
                         jnp.exp(jnp.maximum(diff, 0.0)[None] * lg[:, None, None]), 0.0)
    q_dec = jnp.exp((idx + 1.0)[:, None] * lg[None, :])
    k_dec = jnp.exp((C - 1.0 - idx)[:, None] * lg[None, :])
    c_dec = jnp.exp(C * lg)

    def blocks(a):
        return jnp.moveaxis(a.reshape((B, n, C) + a.shape[2:]), 1, 0)

    def step(s, inp):
        qc, kc, vc = inp
        att = jnp.einsum('bihd,bjhd->bhij', qc, kc) * decay_in[None]
        o = (jnp.einsum('bhij,bjhe->bihe', att, vc)
             + jnp.einsum('bihd,bhde->bihe', qc * q_dec[None, :, :, None], s))
        s = (s * c_dec[None, :, None, None]
             + jnp.einsum('bjhd,bjhe->bhde', kc * k_dec[None, :, :, None], vc))
        return s, o

    s_fin, o = lax.scan(step, s0, (blocks(q), blocks(k), blocks(v)))
    o = jnp.moveaxis(o, 0, 1).reshape(B, T, RET_HEADS, RET_DV)
    return o, s_fin


def mixer_layer(x, pos0, conv_st, ret_st, mem_k, mem_v, norm_mix, w_in, conv_w, conv_b,
                conv_ln_g, conv_ln_b, w_conv_out, ret_gn_g, w_ret_out, w_mem_out, w_out):
    B, T, _ = x.shape
    h = rmsnorm(x, norm_mix)
    z = h @ w_in
    zc, zq, zk, zv, zg, zm, zgate = _split_cols(z)

    a, b = jnp.split(zc, 2, axis=-1)
    u = a * jax.nn.sigmoid(b)
    full = jnp.concatenate([conv_st.astype(u.dtype), u], axis=1)
    new_conv = full[:, -CONV_STATE:]
    c = lax.conv_general_dilated(full, conv_w[:, None, :].astype(full.dtype), (1,), 'VALID',
                                 dimension_numbers=('NWC', 'WIO', 'NWC'),
                                 feature_group_count=CONV_CH) + conv_b
    c = jax.nn.silu(layernorm(c, conv_ln_g, conv_ln_b))
    out_a = c @ w_conv_out

    pos = jnp.arange(T, dtype=jnp.float32) + float(pos0)
    q = rotary(zq.reshape(B, T, RET_HEADS, RET_DK), pos) * (RET_DK ** -0.5)
    k = rotary(zk.reshape(B, T, RET_HEADS, RET_DK), pos)
    v = zv.reshape(B, T, RET_HEADS, RET_DV).astype(jnp.float32)
    o, new_ret = retention(q, k, v, ret_st.astype(jnp.float32))
    mu = jnp.mean(o, axis=-1, keepdims=True)
    oc = o - mu
    o = oc * lax.rsqrt(jnp.mean(oc * oc, axis=-1, keepdims=True) + EPS)
    o = (o.reshape(B, T, RET_HEADS * RET_DV) * ret_gn_g.astype(jnp.float32)).astype(x.dtype)
    out_b = (o * jax.nn.silu(zg)) @ w_ret_out

    qm = zm.reshape(B, T, MEM_HEADS, MEM_DH).astype(jnp.float32)
    sc = jnp.einsum('bthd,bmhd->bhtm', qm, mem_k.astype(jnp.float32)) * (MEM_DH ** -0.5)
    p = jax.nn.softmax(sc, axis=-1)
    om = jnp.einsum('bhtm,bmhd->bthd', p, mem_v.astype(jnp.float32))
    out_c = om.reshape(B, T, MEM_HEADS * MEM_DH).astype(x.dtype) @ w_mem_out

    g = jax.nn.sigmoid(zgate.reshape(B, T, N_BRANCH, D_MODEL))
    merged = g[:, :, 0] * out_a + g[:, :, 1] * out_b + g[:, :, 2] * out_c
    return x + merged @ w_out, new_conv, new_ret.astype(ret_st.dtype)


def peer(h, w_q, subkeys, u_tab, v_tab):
    shape = h.shape
    hf = h.reshape(-1, D_MODEL)
    n_tok = hf.shape[0]
    q = (hf @ w_q).reshape(n_tok, PEER_HEADS, 2, PEER_DQ // 2)
    s = jnp.einsum('nphd,phkd->nphk', q, subkeys.astype(q.dtype)).astype(jnp.float32)
    sv, si = lax.top_k(s, PEER_TOPK)
    cand = (sv[:, :, 0, :, None] + sv[:, :, 1, None, :]).reshape(n_tok, PEER_HEADS, PEER_TOPK * PEER_TOPK)
    cidx = (si[:, :, 0, :, None] * PEER_NKEYS + si[:, :, 1, None, :]).reshape(n_tok, PEER_HEADS, PEER_TOPK * PEER_TOPK)
    tv, tp = lax.top_k(cand, PEER_TOPK)
    eidx = jnp.take_along_axis(cidx, tp, axis=-1)
    gate = jax.nn.softmax(tv, axis=-1).astype(h.dtype)
    pad = (-n_tok) % PEER_BLOCK
    nb = (n_tok + pad) // PEER_BLOCK
    hp = jnp.pad(hf, ((0, pad), (0, 0))).reshape(nb, PEER_BLOCK, D_MODEL)
    ep = jnp.pad(eidx, ((0, pad), (0, 0), (0, 0))).reshape(nb, PEER_BLOCK, PEER_HEADS, PEER_TOPK)
    gp = jnp.pad(gate, ((0, pad), (0, 0), (0, 0))).reshape(nb, PEER_BLOCK, PEER_HEADS, PEER_TOPK)

    def blk(args):
        hb, eb, gb = args
        ub = jnp.take(u_tab, eb, axis=0)
        act = jax.nn.gelu(jnp.einsum('nd,npkd->npk', hb, ub))
        vb = jnp.take(v_tab, eb, axis=0)
        return jnp.einsum('npk,npkd->nd', gb * act, vb)

    out = lax.map(blk, (hp, ep, gp))
    return out.reshape(-1, D_MODEL)[:n_tok].reshape(shape)


def setup_inputs(seed: int = 0) -> dict:
    key = jax.random.key(seed)
    ks = jax.random.split(key, 32)
    L = DEPTH

    def nrm(k, shape, s):
        return jax.random.normal(k, shape, jnp.float32) * s

    return {
        'x_prompt': nrm(ks[0], (BATCH, SEQ, D_MODEL), 1.0),
        'x_sample': nrm(ks[1], (DEC_BATCH, DEC_SEQ, D_MODEL), 1.0),
        'mem_prompt': nrm(ks[2], (BATCH, N_MEM, D_MODEL), 1.0),
        'state_conv': nrm(ks[3], (L, DEC_BATCH, CONV_STATE, CONV_CH), 0.5),
        'state_ret': nrm(ks[4], (L, DEC_BATCH, RET_HEADS, RET_DK, RET_DV), 8.0),
        'cache_mem_k': nrm(ks[5], (L, DEC_BATCH, N_MEM, MEM_HEADS, MEM_DH), 1.0),
        'cache_mem_v': nrm(ks[6], (L, DEC_BATCH, N_MEM, MEM_HEADS, MEM_DH), 1.0),
        'norm_mix': 1.0 + nrm(ks[7], (L, D_MODEL), 0.02),
        'norm_mem': 1.0 + nrm(ks[8], (L, D_MODEL), 0.02),
        'w_in': nrm(ks[9], (L, D_MODEL, N_IN), D_MODEL ** -0.5),
        'conv_w': nrm(ks[10], (L, CONV_WIDTH, CONV_CH), CONV_WIDTH ** -0.5),
        'conv_b': nrm(ks[11], (L, CONV_CH), 0.02),
        'conv_ln_g': 1.0 + nrm(ks[12], (L, CONV_CH), 0.02),
        'conv_ln_b': nrm(ks[13], (L, CONV_CH), 0.02),
        'w_conv_out': nrm(ks[14], (L, CONV_CH, D_MODEL), CONV_CH ** -0.5),
        'ret_gn_g': 1.0 + nrm(ks[15], (L, RET_HEADS * RET_DV), 0.02),
        'w_ret_out': nrm(ks[16], (L, RET_HEADS * RET_DV, D_MODEL), (RET_HEADS * RET_DV) ** -0.5),
        'w_mem_k': nrm(ks[17], (L, D_MODEL, MEM_HEADS * MEM_DH), D_MODEL ** -0.5),
        'w_mem_v': nrm(ks[18], (L, D_MODEL, MEM_HEADS * MEM_DH), D_MODEL ** -0.5),
        'w_mem_out': nrm(ks[19], (L, MEM_HEADS * MEM_DH, D_MODEL), (MEM_HEADS * MEM_DH) ** -0.5),
        'w_out': nrm(ks[20], (L, D_MODEL, D_MODEL), D_MODEL ** -0.5),
        'norm_ffn': 1.0 + nrm(ks[21], (L, D_MODEL), 0.02),
        'w_peer_q': nrm(ks[22], (L, D_MODEL, PEER_HEADS * PEER_DQ), D_MODEL ** -0.5),
        'peer_subkeys': nrm(ks[23], (L, PEER_HEADS, 2, PEER_NKEYS, PEER_DQ // 2), (PEER_DQ // 2) ** -0.5),
        'peer_u': nrm(ks[24], (L, PEER_N, D_MODEL), D_MODEL ** -0.5),
        'peer_v': nrm(ks[25], (L, PEER_N, D_MODEL), PEER_HEADS ** -0.5),
        'norm_final': 1.0 + nrm(ks[26], (D_MODEL,), 0.02),
    }


def reference(x_prompt, x_sample, mem_prompt, state_conv, state_ret, cache_mem_k, cache_mem_v,
              norm_mix, norm_mem, w_in, conv_w, conv_b, conv_ln_g, conv_ln_b, w_conv_out,
              ret_gn_g, w_ret_out, w_mem_k, w_mem_v, w_mem_out, w_out, norm_ffn,
              w_peer_q, peer_subkeys, peer_u, peer_v, norm_final):
    xp, xs = x_prompt, x_sample
    bp = x_prompt.shape[0]
    conv_p, ret_p, memk_p, memv_p, conv_s, ret_s = [], [], [], [], [], []
    for l in range(DEPTH):
        mh = rmsnorm(mem_prompt, norm_mem[l])
        mk = (mh @ w_mem_k[l]).reshape(bp, N_MEM, MEM_HEADS, MEM_DH)
        mv = (mh @ w_mem_v[l]).reshape(bp, N_MEM, MEM_HEADS, MEM_DH)
        conv0 = jnp.zeros((bp, CONV_STATE, CONV_CH), xp.dtype)
        ret0 = jnp.zeros((bp, RET_HEADS, RET_DK, RET_DV), state_ret.dtype)
        lw = (norm_mix[l], w_in[l], conv_w[l], conv_b[l], conv_ln_g[l], conv_ln_b[l], w_conv_out[l],
              ret_gn_g[l], w_ret_out[l], w_mem_out[l], w_out[l])
        xp, cp, rp = mixer_layer(xp, 0, conv0, ret0, mk, mv, *lw)
        xs, cs, rs = mixer_layer(xs, PAST_LEN, state_conv[l], state_ret[l],
                                 cache_mem_k[l], cache_mem_v[l], *lw)
        xp = xp + peer(rmsnorm(xp, norm_ffn[l]), w_peer_q[l], peer_subkeys[l], peer_u[l], peer_v[l])
        xs = xs + peer(rmsnorm(xs, norm_ffn[l]), w_peer_q[l], peer_subkeys[l], peer_u[l], peer_v[l])
        conv_p.append(cp)
        ret_p.append(rp)
        memk_p.append(mk)
        memv_p.append(mv)
        conv_s.append(cs)
        ret_s.append(rs)
    y_prompt = rmsnorm(xp, norm_final)
    y_sample = rmsnorm(xs, norm_final)
    return (y_prompt, y_sample, jnp.stack(conv_p), jnp.stack(ret_p), jnp.stack(memk_p),
            jnp.stack(memv_p), jnp.stack(conv_s), jnp.stack(ret_s))
```

```python
import contextlib
import os
import numpy as np
import concourse.bass as bass
import concourse.mybir as mybir
from concourse.bass_utils import run_bass_kernel_spmd

F32 = mybir.dt.float32
BF16 = mybir.dt.bfloat16
AF = mybir.ActivationFunctionType
ALU = mybir.AluOpType
AX = mybir.AxisListType

NCORES = 8
D = 4096
KT = D // 128
NP = 1024
NS = 64
NTOK = NP + NS
NMEM = 256
EPS = 1e-6
NIN = 23552
C_CONV, C_Q, C_K, C_V, C_G, C_M, C_GATE = 0, 16, 32, 48, 64, 80, 88
TB = [(0, 512), (512, 512), (1024, 64)]
TBP = [(0, 512), (512, 512)]
GAM = [1.0 - 2.0 ** (-5 - h) for h in range(8)]

CO = {}
_o = 0
for _n, _w in (("ident", 128), ("DT", 8 * 128), ("qdec", 8 * 128), ("kdec", 8), ("DTs", 8 * 64),
               ("qdecs", 8 * 64), ("kdecs", 8), ("selq", 16 * 64), ("selk", 16),
               ("convw", 8 * 31), ("convb", 8), ("lng", 8), ("lnb", 8), ("gng", 16)):
    CO[_n] = _o
    _o += _w
CW = _o


class Em:
    NDMA = 12

    def __init__(self, nc, stack):
        self.nc = nc
        self.eng = {"pe": nc.tensor, "act": nc.scalar, "dve": nc.vector,
                    "pool": nc.gpsimd, "sp": nc.sync}
        self.sem = {}
        self.cnt = {}
        for e in ("pe", "act", "dve", "pool"):
            self.sem[e] = stack.enter_context(nc.semaphore("s_" + e))
            self.cnt[e] = 0
        self.dsem = {}
        self.dcnt = {}
        for q in ("sp", "act", "pool"):
            self.dsem[q] = [stack.enter_context(nc.semaphore("d_%s%d" % (q, i)))
                            for i in range(self.NDMA)]
            self.dcnt[q] = 0
        self.waited = {}
        self.last_w = {}
        self.readers = {}
        self.n_inst = 0

    def _wait(self, e, tok):
        sem, val, prod = tok
        if prod == "pe" and e == "pe":
            return
        k = (e, id(sem))
        if self.waited.get(k, 0) >= val:
            return
        self.eng[e].wait_ge(sem, val)
        self.waited[k] = val
        self.n_inst += 1

    def _deps(self, e, reads, writes):
        for k in reads:
            t = self.last_w.get(k)
            if t is not None:
                self._wait(e, t)
        for k in writes:
            t = self.last_w.get(k)
            if t is not None:
                self._wait(e, t)
            for t in self.readers.get(k, ()):
                self._wait(e, t)

    def _commit(self, tok, reads, writes):
        for k in reads:
            self.readers.setdefault(k, []).append(tok)
        for k in writes:
            self.last_w[k] = tok
            self.readers[k] = []

    dead = False

    def op(self, e, fn, reads=(), writes=()):
        if self.dead:
            return None
        px = [k for k in reads if isinstance(k, str) and k.startswith("ps")]
        if px:
            reads = [k for k in reads if k not in px]
            writes = list(writes) + px
        self._deps(e, reads, writes)
        ins = fn(self.eng[e])
        self.cnt[e] += 1
        ins.then_inc(self.sem[e], 1)
        tok = (self.sem[e], self.cnt[e], e)
        self._commit(tok, reads, writes)
        self.n_inst += 1
        return tok

    def dma(self, q, out, in_, reads=(), writes=(), **kw):
        if self.dead:
            return None
        i = self.dcnt[q]
        sem = self.dsem[q][i % self.NDMA]
        rnd = i // self.NDMA
        if rnd > 0:
            self._wait(q, (sem, rnd * 16, None))
        self._deps(q, reads, writes)
        ins = self.eng[q].dma_start(out=out, in_=in_, **kw)
        ins.then_inc(sem, 16)
        self.dcnt[q] = i + 1
        tok = (sem, (rnd + 1) * 16, None)
        self._commit(tok, reads, writes)
        self.n_inst += 1
        return tok

    def _all_tokens(self):
        toks = []
        for q in self.dsem:
            n = self.dcnt[q]
            for j in range(min(n, self.NDMA)):
                last_i = j + ((n - 1 - j) // self.NDMA) * self.NDMA
                toks.append((self.dsem[q][j], (last_i // self.NDMA + 1) * 16, None))
        for e in ("pe", "act", "dve", "pool"):
            if self.cnt[e]:
                toks.append((self.sem[e], self.cnt[e], e))
        return toks

    def barrier(self):
        if self.dead:
            return
        if os.environ.get("TG_NOBAR", "0") == "1":
            return
        toks = self._all_tokens()
        for e in ("pe", "act", "dve", "pool", "sp"):
            for t in toks:
                if t[2] == e:
                    continue
                self._wait(e, t)
        self.last_w.clear()
        self.readers.clear()

    def finish(self):
        self.dead = False
        for t in self._all_tokens():
            self._wait("sp", t)


def build(stop_after="all", dbg=(), _pre=None):
    if _pre is None:
        _nc0 = build(stop_after, dbg, _pre=())
        _pre = tuple(_nc0.used_inputs.keys())
    nc = bass.Bass("TRN2", target_bir_lowering=False)
    uid = [0]

    def din(name, shape, dt=F32):
        return nc.dram_tensor(name, list(shape), dt, kind="ExternalInput").ap()

    def dout(name, shape, dt=F32):
        return nc.dram_tensor(name, list(shape), dt, kind="ExternalOutput").ap()

    def dscr(name, shape, dt):
        return nc.dram_tensor(name, list(shape), dt, kind=("ExternalOutput" if (dbg and name in dbg) else "Internal")).ap()

    in_shapes = {
        "xm": [NTOK, D], "xpre": [NP, D], "mem": [NMEM, D], "st_conv": [16, 30, 1024],
        "st_ret": [16, 8, 256, 256], "c_mk": [16, 256, 1024], "c_mv": [16, 256, 1024],
        "norm_mix": [1, D], "norm_mem": [1, D], "norm_ffn": [1, D], "norm_final": [1, D],
        "w_in": [D, NIN], "w_conv_out": [1024, D], "w_ret_out": [2048, D], "w_mem_k": [D, 1024],
        "w_mem_v": [D, 1024], "w_mem_out": [1024, D], "w_out": [D, D], "w_peer_q": [D, 2048],
        "peer_sk": [16 * 128, 128], "peer_u": [16384, D], "peer_v": [16384, D],
        "cst": [128, CW], "rot": [128, 2, 2112],
    }
    declared = {}

    def I(name):
        if name not in declared:
            declared[name] = din(name, in_shapes[name])
        return declared[name]

    nc.used_inputs = declared
    for _n in _pre:
        I(_n)

    y_o = dout("y_o", [NTOK, D])
    convp_o = dout("convp_o", [32, 1024])
    retp_o = dout("retp_o", [8, 256, 256])
    memk_o = dout("memk_o", [NMEM, 1024])
    memv_o = dout("memv_o", [NMEM, 1024])
    convs_o = dout("convs_o", [16, 30, 1024])
    rets_o = dout("rets_o", [16, 8, 256, 256])

    zT = dscr("zT", [NIN, NTOK], BF16)
    zP = dscr("zP", [48 * 128, NP], BF16)
    mT = dscr("mT", [D, NTOK], BF16)
    x1s = dscr("x1s", [NTOK, D], F32)
    x2s = dscr("x2s", [NTOK, D], F32)
    Wd = dscr("Wd", [NTOK, 16384], BF16)
    TTd = dscr("TTd", [16384, NTOK], BF16)

    with contextlib.ExitStack() as st:
        em = Em(nc, st)

        def sbt(stack, name, shape, dt):
            uid[0] += 1
            return stack.enter_context(nc.sbuf_tensor("%s_%d" % (name, uid[0]), list(shape), dt))

        if os.environ.get("TG_PS", "0") == "1":
            PSB = [st.enter_context(nc.psum_tensor("PS%d" % i, [128, 512], F32)) for i in range(8)]

            def bank(b, n=512, off=0):
                return PSB[b][:, off: off + n]

            def bankbf(b):
                return PSB[b].bitcast(BF16)
        else:
            PS = st.enter_context(nc.psum_tensor("PS", [128, 4096], F32))

            def bank(b, n=512, off=0):
                return PS[:, b * 512 + off: b * 512 + off + n]

            def bankbf(b):
                return PS[:, b * 512:(b + 1) * 512].bitcast(BF16)

        cst = sbt(st, "cst", [128, CW], F32)
        em.dma("sp", cst[:], I("cst"), writes=["cst"])
        ident_f = cst[:, CO["ident"]:CO["ident"] + 128]
        if os.environ.get("TG_IDENT", "0") == "1":
            ident_ft = sbt(st, "ident_ft", [128, 128], F32)
            em.dma("sp", ident_ft[:], I("cst")[:, CO["ident"]:CO["ident"] + 128], writes=["cst2"])
            em.barrier()
            ident_f = ident_ft[:]
        ident_b = sbt(st, "ident_b", [128, 128], BF16)
        em.op("dve", lambda e: e.tensor_copy(out=ident_b[:], in_=ident_f), reads=["cst"], writes=["ident_b"])
        eps_t = sbt(st, "eps_t", [128, 1], F32)
        em.op("dve", lambda e: e.memset(eps_t[:], EPS), writes=["eps"])
        ones_f = sbt(st, "ones_f", [128, 128], F32)
        em.op("dve", lambda e: e.memset(ones_f[:], 1.0), writes=["ones_f"])
        mkT = sbt(st, "mkT", [128, 8, NMEM], BF16)
        mv_tm = sbt(st, "mv_tm", [128, 2, 1024], BF16)

        class _Stop(Exception):
            pass

        def stop_pt(n):
            if os.environ.get("TG_STOP", "0") == str(n):
                em.barrier()
                em.dead = True

        def cc_(name, j=None, w=1):
            o = CO[name] + (0 if j is None else j * w)
            return cst[:, o:o + w]

        def prep(ph, x_dram, nrows, g_dram, hT, tok0):
            gbc = sbt(ph, "gbc", [128, D], F32)
            xt = [sbt(ph, "xt", [128, D], F32) for _ in range(2)]
            hb = [sbt(ph, "hb", [128, D], BF16) for _ in range(2)]
            stat = [sbt(ph, "stat", [128, 4], F32) for _ in range(2)]
            junk = sbt(ph, "junk", [128, D], BF16)
            em.dma("sp", gbc[:], g_dram.broadcast_to([128, D]), writes=["gbc"])
            ntile = (nrows + 127) // 128
            for t in range(ntile):
                r = min(128, nrows - t * 128)
                b = t % 2
                em.dma("sp", xt[b][:r, :], x_dram[t * 128:t * 128 + r, :], writes=["xt%d" % b])
                em.op("act", lambda e: e.activation(out=junk[:r, :], in_=xt[b][:r, :], func=AF.Square,
                                                    accum_out=stat[b][:r, 0:1]),
                      reads=["xt%d" % b], writes=["junk", "st%d" % b])
                em.op("act", lambda e: e.activation(out=stat[b][:r, 1:2], in_=stat[b][:r, 0:1], func=AF.Sqrt,
                                                    bias=eps_t[:r, :], scale=1.0 / D),
                      reads=["st%d" % b, "eps"], writes=["st%d" % b])
                em.op("dve", lambda e: e.reciprocal(out=stat[b][:r, 2:3], in_=stat[b][:r, 1:2]),
                      reads=["st%d" % b], writes=["st%d" % b])
                em.op("dve", lambda e: e.scalar_tensor_tensor(out=hb[b][:r, :], in0=xt[b][:r, :],
                                                              scalar=stat[b][:r, 2:3], in1=gbc[:r, :],
                                                              op0=ALU.mult, op1=ALU.mult),
                      reads=["xt%d" % b, "st%d" % b, "gbc"], writes=["hb%d" % b])
                for g in range(4):
                    pb = 6 + (g % 2)
                    pst = bankbf(pb)
                    for i in range(8):
                        kt = g * 8 + i
                        em.op("pe", lambda e: e.transpose(out=pst[:, i * 128:i * 128 + r],
                                                          in_=hb[b][:r, kt * 128:(kt + 1) * 128],
                                                          identity=ident_b[:r, :r]),
                              reads=["hb%d" % b, "ident_b"], writes=["ps%d" % pb])
                    src = pst.rearrange("p (i c) -> p i c", i=8)[:, :, :r]
                    dst = hT[:, g * 8:(g + 1) * 8, tok0 + t * 128: tok0 + t * 128 + r]
                    if g % 2 == 0:
                        em.op("act", lambda e: e.copy(out=dst, in_=src), reads=["ps%d" % pb], writes=["hT"])
                    else:
                        em.op("dve", lambda e: e.tensor_copy(out=dst, in_=src), reads=["ps%d" % pb], writes=["hT"])

        lin_n = [0]

        def linear_fm(wbf, hT, hkeys, tok_blocks, w_dram, nk, col_chunks, consumer, k0=0):
            wv = w_dram.rearrange("(kt p) n -> p kt n", p=128)
            nb = len(wbf)
            assert len(col_chunks) % 2 == 0
            for pi in range(len(col_chunks) // 2):
                j0 = col_chunks[2 * pi]
                assert col_chunks[2 * pi + 1] == j0 + 1
                s = lin_n[0] % nb
                em.dma("pool", wbf[s][:, :nk, :], wv[:, :, j0 * 128:(j0 + 2) * 128], writes=["wbf%d" % s])
                for sub in range(2):
                    idx = 2 * pi + sub
                    j = j0 + sub
                    n = lin_n[0] * 2 + sub
                    base = (n % 2) * 3
                    pss, keys = [], []
                    for bi, (t0, tn) in enumerate(tok_blocks):
                        pb = base + bi
                        for kt in range(nk):
                            em.op("pe", lambda e: e.matmul(bank(pb, tn), lhsT=wbf[s][:, kt, sub * 128:(sub + 1) * 128],
                                                           rhs=hT[:, k0 + kt, t0:t0 + tn],
                                                           start=(kt == 0), stop=(kt == nk - 1)),
                                  reads=["wbf%d" % s] + list(hkeys), writes=["ps%d" % pb])
                        pss.append(bank(pb, tn))
                        keys.append("ps%d" % pb)
                    consumer(idx, j, pss, keys)
                lin_n[0] += 1

        try:
          stop_pt(1)
          with contextlib.ExitStack() as ph:
            memT = sbt(ph, "memT", [128, KT, NMEM], BF16)
            with contextlib.ExitStack() as ph2:
                prep(ph2, I("mem"), NMEM, I("norm_mem"), memT, 0)
                em.barrier()
            stop_pt(2)
            wbf = [sbt(ph, "wbf", [128, KT, 256], BF16) for _ in range(3)]
            mkT_f = sbt(ph, "mkT_f", [128, 8, NMEM], F32)
            otile = [sbt(ph, "otile", [128, 1024], F32) for _ in range(2)]
            for wi, (wd, od) in enumerate(((I("w_mem_k"), memk_o), (I("w_mem_v"), memv_o))):
                def cons(idx, j, pss, keys):
                    em.op("act", lambda e: e.copy(out=mkT_f[:, idx, :], in_=pss[0]),
                          reads=keys, writes=["mkT_f"])
                    if wi == 0 and os.environ.get("TG_NOMKT", "0") != "1":
                        em.op("dve", lambda e: e.tensor_copy(out=mkT[:, idx, :], in_=pss[0]),
                              reads=keys, writes=["mkT"])
                linear_fm(wbf, memT, ["hT"], [(0, NMEM)], wd, KT, list(range(8)), cons)
                for t in range(2):
                    for g in range(2):
                        pb = 6 + g
                        for i in range(4):
                            jj = g * 4 + i
                            em.op("pe", lambda e: e.transpose(out=bank(pb, 128, i * 128),
                                                              in_=mkT_f[:, jj, t * 128:(t + 1) * 128],
                                                              identity=ident_f),
                                  reads=["mkT_f", "cst"], writes=["ps%d" % pb])
                        em.op("dve", lambda e: e.tensor_copy(out=otile[t][:, g * 512:(g + 1) * 512], in_=bank(pb)),
                              reads=["ps%d" % pb], writes=["otile%d" % t])
                        if wi == 1:
                            em.op("act", lambda e: e.copy(out=mv_tm[:, t, g * 512:(g + 1) * 512], in_=bank(pb)),
                                  reads=["ps%d" % pb], writes=["mv_tm"])
                    em.dma(os.environ.get("TG_OUTQ", "sp"), od[t * 128:(t + 1) * 128, :], otile[t][:], reads=["otile%d" % t],
                           writes=["out_%d_%d" % (wi, t)])
                stop_pt(3 + wi)
            em.barrier()
        except _Stop:
            pass

        if stop_after == "M":
            em.finish()
            print("instructions emitted:", em.n_inst)
            return nc

        with contextlib.ExitStack() as ph:
            hT = sbt(ph, "hT", [128, KT, NTOK], BF16)
            for phase in ("pre", "main"):
                with contextlib.ExitStack() as ph2:
                    if phase == "pre":
                        prep(ph2, I("xpre"), NP, I("norm_mix"), hT, 0)
                    else:
                        prep(ph2, I("xm"), NTOK, I("norm_mix"), hT, 0)
                    em.barrier()
                with contextlib.ExitStack() as ph2:
                    wbf = [sbt(ph2, "wbf", [128, KT, 256], BF16) for _ in range(3)]
                    zt = [sbt(ph2, "zt", [128, NTOK], BF16) for _ in range(3)]
                    if phase == "pre":
                        chunks = list(range(0, 16)) + list(range(32, 64))
                        tbs, ntk, zdst = TBP, NP, zP
                    else:
                        chunks = list(range(184))
                        tbs, ntk, zdst = TB, NTOK, zT

                    def cons(idx, j, pss, keys):
                        zi = idx % 3
                        if 8 <= j < 16 or j >= C_GATE:
                            fn = AF.Sigmoid
                        elif C_G <= j < C_M:
                            fn = AF.Silu
                        else:
                            fn = AF.Copy
                        for (t0, tn), p_, k_ in zip(tbs, pss, keys):
                            em.op("act", lambda e: e.activation(out=zt[zi][:, t0:t0 + tn], in_=p_, func=fn),
                                  reads=[k_], writes=["zt%d" % zi])
                        em.dma("sp", zdst[idx * 128:(idx + 1) * 128, :] if phase == "pre"
                               else zdst[j * 128:(j + 1) * 128, :],
                               zt[zi][:, :ntk], reads=["zt%d" % zi], writes=["zscr"])
                    linear_fm(wbf, hT, ["hT"], tbs, I("w_in"), KT, chunks, cons)
                    em.barrier()
                if stop_after == "pre":
                    break

        if stop_after in ("z", "pre"):
            em.finish()
            print("instructions emitted:", em.n_inst)
            return nc

        br = contextlib.ExitStack()
        cnT = sbt(br, "cnT", [128, 8, NTOK], BF16)

        with contextlib.ExitStack() as ph:
            cc = sbt(ph, "cc", [128, 8, NTOK], F32)
            us_all = sbt(ph, "us_all", [128, 8, 16, 34], F32)
            usn = sbt(ph, "usn", [128, 8, NS], F32)
            utail = sbt(ph, "utail", [128, 8, 32], F32)
            stt = [sbt(ph, "stt", [120, 1024], F32) for _ in range(2)]
            for g in range(4):
                b = g % 2
                em.dma("sp", stt[b][:, :], I("st_conv")[4 * g:4 * g + 4].rearrange("s t c -> (s t) c"),
                       writes=["stt%d" % b])
                for j in range(8):
                    pb = 6 + (j % 2)
                    em.op("pe", lambda e: e.transpose(out=bank(pb, 120), in_=stt[b][:, j * 128:(j + 1) * 128],
                                                      identity=ident_f[:120, :120]),
                          reads=["stt%d" % b, "cst"], writes=["ps%d" % pb])
                    em.op("act", lambda e: e.copy(out=us_all[:, j, 4 * g:4 * g + 4, 0:30],
                                                  in_=bank(pb, 120).rearrange("p (s t) -> p s t", s=4)),
                          reads=["ps%d" % pb], writes=["us_all"])
            em.dma("sp", convs_o[:, 0:26, :], I("st_conv")[:, 4:30, :], writes=["convs_a"])
            at = [sbt(ph, "at", [128, 1054], BF16) for _ in range(2)]
            sgt = [sbt(ph, "sgt", [128, 1054], BF16) for _ in range(2)]
            as_ = [sbt(ph, "as", [128, NS], BF16) for _ in range(2)]
            sgs = [sbt(ph, "sgs", [128, NS], BF16) for _ in range(2)]
            ut = [sbt(ph, "ut", [128, 1054], F32) for _ in range(2)]
            for j in range(8):
                b = j % 2
                ka = "cin%d" % b
                em.dma("sp", at[b][:, 0:30], zP[j * 128:(j + 1) * 128, 994:1024], reads=["zscr"], writes=[ka + "a"])
                em.dma("sp", at[b][:, 30:1054], zT[j * 128:(j + 1) * 128, 0:NP], reads=["zscr"], writes=[ka + "a"])
                em.dma("sp", sgt[b][:, 0:30], zP[(8 + j) * 128:(9 + j) * 128, 994:1024], reads=["zscr"], writes=[ka + "b"])
                em.dma("sp", sgt[b][:, 30:1054], zT[(8 + j) * 128:(9 + j) * 128, 0:NP], reads=["zscr"], writes=[ka + "b"])
                em.dma("sp", as_[b][:, :], zT[j * 128:(j + 1) * 128, NP:NTOK], reads=["zscr"], writes=[ka + "c"])
                em.dma("sp", sgs[b][:, :], zT[(8 + j) * 128:(9 + j) * 128, NP:NTOK], reads=["zscr"], writes=[ka + "d"])
                em.op("dve", lambda e: e.tensor_tensor(out=ut[b][:, :], in0=at[b][:, :], in1=sgt[b][:, :], op=ALU.mult),
                      reads=[ka + "a", ka + "b"], writes=["ut%d" % b])
                em.op("dve", lambda e: e.tensor_tensor(out=usn[:, j, :], in0=as_[b][:, :], in1=sgs[b][:, :], op=ALU.mult),
                      reads=[ka + "c", ka + "d"], writes=["usn"])
                em.op("act", lambda e: e.copy(out=us_all[:, j, :, 30:34],
                                              in_=usn[:, j, :].rearrange("p (t s) -> p s t", t=4)),
                      reads=["usn"], writes=["us_all"])
                em.op("act", lambda e: e.copy(out=utail[:, j, :], in_=ut[b][:, 1022:1054]),
                      reads=["ut%d" % b], writes=["utail"])
                cw0 = CO["convw"] + j * 31
                acc = cc[:, j, 0:NP]
                em.op("dve", lambda e: e.tensor_scalar(out=acc, in0=ut[b][:, 0:NP], scalar1=cst[:, cw0:cw0 + 1],
                                                       scalar2=cst[:, CO["convb"] + j:CO["convb"] + j + 1],
                                                       op0=ALU.mult, op1=ALU.add),
                      reads=["ut%d" % b, "cst"], writes=["cc"])
                for k in range(1, 31):
                    em.op("dve", lambda e: e.scalar_tensor_tensor(out=acc, in0=ut[b][:, k:k + NP],
                                                                  scalar=cst[:, cw0 + k:cw0 + k + 1], in1=acc,
                                                                  op0=ALU.mult, op1=ALU.add),
                          reads=["ut%d" % b, "cst"], writes=["cc"])
                accs = cc[:, j, NP:NTOK].rearrange("p (t s) -> p s t", t=4)
                em.op("dve", lambda e: e.tensor_scalar(out=accs, in0=us_all[:, j, :, 0:4], scalar1=cst[:, cw0:cw0 + 1],
                                                       scalar2=cst[:, CO["convb"] + j:CO["convb"] + j + 1],
                                                       op0=ALU.mult, op1=ALU.add),
                      reads=["us_all", "cst"], writes=["cc"])
                for k in range(1, 31):
                    em.op("dve", lambda e: e.scalar_tensor_tensor(out=accs, in0=us_all[:, j, :, k:k + 4],
                                                                  scalar=cst[:, cw0 + k:cw0 + k + 1], in1=accs,
                                                                  op0=ALU.mult, op1=ALU.add),
                          reads=["us_all", "cst"], writes=["cc"])
            cpt = sbt(ph, "cpt", [32, 1024], F32)
            cst_t = sbt(ph, "cst_t", [64, 1024], F32)
            for g in range(2):
                pb = 6 + g
                for i in range(4):
                    j = g * 4 + i
                    em.op("pe", lambda e: e.transpose(out=bank(pb, 128, i * 128)[:32, :], in_=utail[:, j, :],
                                                      identity=ident_f),
                          reads=["utail", "cst"], writes=["ps%d" % pb])
                em.op("dve", lambda e: e.tensor_copy(out=cpt[:, g * 512:(g + 1) * 512], in_=bank(pb)[:32, :]),
                      reads=["ps%d" % pb], writes=["cpt"])
            em.dma("sp", convp_o, cpt[:, :], reads=["cpt"], writes=["convp_o"])
            for g in range(2):
                pb = 6 + g
                for i in range(4):
                    j = g * 4 + i
                    em.op("pe", lambda e: e.transpose(out=bank(pb, 128, i * 128)[:64, :], in_=usn[:, j, :],
                                                      identity=ident_f),
                          reads=["usn", "cst"], writes=["ps%d" % pb])
                em.op("dve", lambda e: e.tensor_copy(out=cst_t[:, g * 512:(g + 1) * 512], in_=bank(pb)[:64, :]),
                      reads=["ps%d" % pb], writes=["cst_t"])
            for t in range(4):
                em.dma("sp", convs_o[:, 26 + t, :], cst_t[t * 16:(t + 1) * 16, :], reads=["cst_t"],
                       writes=["convs_b%d" % t])

            sq = [sbt(ph, "sq", [128, NTOK], F32) for _ in range(2)]
            for j in range(8):
                b = j % 2
                em.op("act", lambda e: e.activation(out=sq[b][:, :], in_=cc[:, j, :], func=AF.Square),
                      reads=["cc"], writes=["sq%d" % b])
                for bi, (t0, tn) in enumerate(TB):
                    em.op("pe", lambda e: e.matmul(bank(bi, tn), lhsT=ones_f[:], rhs=cc[:, j, t0:t0 + tn],
                                                   start=(j == 0), stop=(j == 7)),
                          reads=["cc", "ones_f"], writes=["ps%d" % bi])
                    em.op("pe", lambda e: e.matmul(bank(3 + bi, tn), lhsT=ones_f[:], rhs=sq[b][:, t0:t0 + tn],
                                                   start=(j == 0), stop=(j == 7)),
                          reads=["sq%d" % b, "ones_f"], writes=["ps%d" % (3 + bi)])
            mu = sbt(ph, "mu", [128, NTOK], F32)
            rs = sbt(ph, "rs", [128, NTOK], F32)
            for bi, (t0, tn) in enumerate(TB):
                em.op("act", lambda e: e.activation(out=mu[:, t0:t0 + tn], in_=bank(bi, tn), func=AF.Copy,
                                                    scale=1.0 / 1024),
                      reads=["ps%d" % bi], writes=["mu"])
                em.op("dve", lambda e: e.tensor_tensor(out=rs[:, t0:t0 + tn], in0=mu[:, t0:t0 + tn],
                                                       in1=mu[:, t0:t0 + tn], op=ALU.mult),
                      reads=["mu"], writes=["rs"])
                em.op("dve", lambda e: e.scalar_tensor_tensor(out=rs[:, t0:t0 + tn], in0=bank(3 + bi, tn),
                                                              scalar=1.0 / 1024, in1=rs[:, t0:t0 + tn],
                                                              op0=ALU.mult, op1=ALU.subtract),
                      reads=["ps%d" % (3 + bi), "rs"], writes=["rs"])
            em.op("act", lambda e: e.activation(out=rs[:, :], in_=rs[:, :], func=AF.Sqrt, bias=eps_t[:, :], scale=1.0),
                  reads=["rs", "eps"], writes=["rs"])
            em.op("dve", lambda e: e.reciprocal(out=rs[:, :], in_=rs[:, :]), reads=["rs"], writes=["rs"])
            for j in range(8):
                b = j % 2
                em.op("dve", lambda e: e.tensor_tensor(out=sq[b][:, :], in0=cc[:, j, :], in1=mu[:, :], op=ALU.subtract),
                      reads=["cc", "mu"], writes=["sq%d" % b])
                em.op("dve", lambda e: e.tensor_tensor(out=sq[b][:, :], in0=sq[b][:, :], in1=rs[:, :], op=ALU.mult),
                      reads=["sq%d" % b, "rs"], writes=["sq%d" % b])
                em.op("act", lambda e: e.activation(out=cnT[:, j, :], in_=sq[b][:, :], func=AF.Silu,
                                                    scale=cst[:, CO["lng"] + j:CO["lng"] + j + 1],
                                                    bias=cst[:, CO["lnb"] + j:CO["lnb"] + j + 1]),
                      reads=["sq%d" % b, "cst"], writes=["cnT"])
            em.barrier()

        if stop_after == "conv":
            em.finish()
            print("instructions emitted:", em.n_inst)
            return nc

        ogT = sbt(br, "ogT", [128, 16, NTOK], BF16)
        with contextlib.ExitStack() as ph:
            rot = sbt(ph, "rot", [128, 2, 2112], F32)
            em.dma("sp", rot[:], I("rot"), writes=["rot"])
            qT = [sbt(ph, "qT", [128, 2, NTOK], BF16)] * 2
            kT = [sbt(ph, "kT", [128, 2, 2112], BF16)] * 2
            vT = [sbt(ph, "vT", [128, 2, 2112], BF16)] * 2
            zg = [sbt(ph, "zg", [128, 2, NTOK], BF16)] * 2
            qR = sbt(ph, "qR", [128, 2, NTOK], BF16)
            qS = sbt(ph, "qS", [128, 2, NTOK], BF16)
            kR = sbt(ph, "kR", [128, 2, 2112], BF16)
            ta = sbt(ph, "ta", [128, 2112], BF16)
            tb_ = sbt(ph, "tb", [128, 2112], BF16)
            tc_ = sbt(ph, "tc", [128, NTOK], BF16)
            td_ = sbt(ph, "td", [128, NTOK], BF16)
            S = sbt(ph, "S", [128, 2, 256], F32)
            S_bf = sbt(ph, "S_bf", [128, 2, 256], BF16)
            k_tm = [sbt(ph, "k_tm", [128, 256], BF16) for _ in range(2)]
            v_tm = [sbt(ph, "v_tm", [128, 256], BF16) for _ in range(2)]
            attb = [sbt(ph, "attb", [128, 128], BF16) for _ in range(2)]
            o_all = sbt(ph, "o_all", [128, 9, 256], F32)
            o_sq = sbt(ph, "o_sq", [128, 256], BF16)
            gst = sbt(ph, "gst", [128, 5, 9], F32)
            on = [sbt(ph, "on", [128, 256], BF16) for _ in range(2)]
            o_st = sbt(ph, "o_st", [64, 256], F32)
            Ss = [sbt(ph, "Ss", [128, 2, 256], F32) for _ in range(3)]
            Sn = [sbt(ph, "Sn", [128, 2, 256], F32) for _ in range(2)]
            qSm = sbt(ph, "qSm", [128, 2, 16, NS], F32)
            kmask = sbt(ph, "kmask", [64, 16, 256], BF16)
            em.op("dve", lambda e: e.memset(o_all[:], 0.0), writes=["o_all"])
            cosr, sinr = rot[:, 0, :], rot[:, 1, :]

            def rotary(eng, src, dst, c0, n, t1, t2):
                cs, sn = cosr[:, c0:c0 + n], sinr[:, c0:c0 + n]
                x1, x2 = src[:, 0, 0:n], src[:, 1, 0:n]
                kk = ["rot", "rsrc"]
                em.op(eng, lambda e: e.tensor_tensor(out=t1[:, 0:n], in0=x1, in1=cs, op=ALU.mult), reads=kk, writes=[eng + "t1"])
                em.op(eng, lambda e: e.tensor_tensor(out=t2[:, 0:n], in0=x2, in1=sn, op=ALU.mult), reads=kk, writes=[eng + "t2"])
                em.op(eng, lambda e: e.tensor_tensor(out=dst[:, 0, 0:n], in0=t1[:, 0:n], in1=t2[:, 0:n], op=ALU.subtract),
                      reads=[eng + "t1", eng + "t2"], writes=["rdst" + eng])
                em.op(eng, lambda e: e.tensor_tensor(out=t1[:, 0:n], in0=x1, in1=sn, op=ALU.mult), reads=kk, writes=[eng + "t1"])
                em.op(eng, lambda e: e.tensor_tensor(out=t2[:, 0:n], in0=x2, in1=cs, op=ALU.mult), reads=kk, writes=[eng + "t2"])
                em.op(eng, lambda e: e.tensor_tensor(out=dst[:, 1, 0:n], in0=t1[:, 0:n], in1=t2[:, 0:n], op=ALU.add),
                      reads=[eng + "t1", eng + "t2"], writes=["rdst" + eng])

            for h in range(8):
                b = 0
                ld = "ld%d" % b
                for half in range(2):
                    em.dma("sp", qT[b][:, half, :], zT[(C_Q + h * 2 + half) * 128:(C_Q + h * 2 + half + 1) * 128, :],
                           reads=["zscr"], writes=[ld])
                    em.dma("sp", kT[b][:, half, 0:NP], zP[(16 + h * 2 + half) * 128:(17 + h * 2 + half) * 128, :],
                           reads=["zscr"], writes=[ld])
                    em.dma("sp", kT[b][:, half, NP:2112], zT[(C_K + h * 2 + half) * 128:(C_K + h * 2 + half + 1) * 128, :],
                           reads=["zscr"], writes=[ld])
                    em.dma("sp", vT[b][:, half, 0:NP], zP[(32 + h * 2 + half) * 128:(33 + h * 2 + half) * 128, :],
                           reads=["zscr"], writes=[ld])
                    em.dma("sp", vT[b][:, half, NP:2112], zT[(C_V + h * 2 + half) * 128:(C_V + h * 2 + half + 1) * 128, :],
                           reads=["zscr"], writes=[ld])
                    em.dma("sp", zg[b][:, half, :], zT[(C_G + h * 2 + half) * 128:(C_G + h * 2 + half + 1) * 128, :],
                           reads=["zscr"], writes=[ld])
                em.op("dve", lambda e: e.tensor_copy(out=ta[:, 0:1], in_=kT[b][:, 0, 0:1]), reads=[ld], writes=["rsrc"])
                rotary("dve", kT[b], kR, 0, 2112, ta, tb_)
                rotary("pool", qT[b], qR, NP, NTOK, tc_, td_)
                qd = cst[:, CO["qdec"] + h * 128:CO["qdec"] + (h + 1) * 128]
                qds = cst[:, CO["qdecs"] + h * 64:CO["qdecs"] + (h + 1) * 64]
                for half in range(2):
                    em.op("pool", lambda e: e.tensor_tensor(
                        out=qS[:, half, 0:NP].rearrange("p (c i) -> p c i", i=128),
                        in0=qR[:, half, 0:NP].rearrange("p (c i) -> p c i", i=128),
                        in1=qd.unsqueeze(1).broadcast_to([128, 8, 128]), op=ALU.mult),
                        reads=["rdstpool", "cst"], writes=["qS"])
                    em.op("pool", lambda e: e.tensor_tensor(out=qS[:, half, NP:NTOK], in0=qR[:, half, NP:NTOK],
                                                            in1=qds, op=ALU.mult),
                          reads=["rdstpool", "cst"], writes=["qS"])
                em.op("dve", lambda e: e.memset(S[:], 0.0), writes=["S"])
                em.op("act", lambda e: e.copy(out=S_bf[:], in_=S[:]), reads=["S"], writes=["S_bf"])
                kd = cst[:, CO["kdec"] + h:CO["kdec"] + h + 1]
                DTh = cst[:, CO["DT"] + h * 128:CO["DT"] + (h + 1) * 128]
                cdec = GAM[h] ** 128
                pst0 = bankbf(0)
                for c in range(16):
                    cb = c % 2
                    cols = slice(c * 128, (c + 1) * 128)
                    for half in range(2):
                        em.op("pe", lambda e: e.transpose(out=pst0[:, half * 128:(half + 1) * 128], in_=kR[:, half, cols],
                                                          identity=ident_b[:]),
                              reads=["rdstdve", "ident_b"], writes=["ps0"])
                        em.op("pe", lambda e: e.transpose(out=pst0[:, 256 + half * 128:256 + (half + 1) * 128],
                                                          in_=vT[b][:, half, cols], identity=ident_b[:]),
                              reads=[ld, "ident_b"], writes=["ps0"])
                    em.op("dve", lambda e: e.tensor_scalar(out=k_tm[cb][:, :], in0=pst0[:, 0:256], scalar1=kd, scalar2=None,
                                                           op0=ALU.mult),
                          reads=["ps0", "cst"], writes=["k_tm%d" % cb])
                    em.op("act", lambda e: e.copy(out=v_tm[cb][:, :], in_=pst0[:, 256:512]),
                          reads=["ps0"], writes=["v_tm%d" % cb])
                    if c >= 8:
                        i = c - 8
                        qc = slice(i * 128, (i + 1) * 128)
                        for half in range(2):
                            em.op("pe", lambda e: e.matmul(bank(1, 128), lhsT=kR[:, half, cols], rhs=qR[:, half, qc],
                                                           start=(half == 0), stop=(half == 1)),
                                  reads=["rdstdve", "rdstpool"], writes=["ps1"])
                        em.op("dve", lambda e: e.tensor_tensor(out=attb[cb][:, :], in0=bank(1, 128), in1=DTh, op=ALU.mult),
                              reads=["ps1", "cst"], writes=["attb%d" % cb])
                        ob = 2 + (i % 2)
                        em.op("pe", lambda e: e.matmul(bank(ob, 256), lhsT=attb[cb][:, :], rhs=v_tm[cb][:, :],
                                                       start=True, stop=False),
                              reads=["attb%d" % cb, "v_tm%d" % cb], writes=["ps%d" % ob])
                        for half in range(2):
                            em.op("pe", lambda e: e.matmul(bank(ob, 256), lhsT=qS[:, half, qc], rhs=S_bf[:, half, :],
                                                           start=False, stop=(half == 1)),
                                  reads=["qS", "S_bf"], writes=["ps%d" % ob])
                        em.op("act", lambda e: e.copy(out=o_all[:, i, :], in_=bank(ob, 256)),
                              reads=["ps%d" % ob], writes=["o_all"])
                    for half in range(2):
                        em.op("pe", lambda e: e.matmul(bank(4 + half, 256), lhsT=k_tm[cb][:, half * 128:(half + 1) * 128],
                                                       rhs=v_tm[cb][:, :], start=True, stop=True),
                              reads=["k_tm%d" % cb, "v_tm%d" % cb], writes=["ps%d" % (4 + half)])
                        em.op("dve", lambda e: e.scalar_tensor_tensor(out=S[:, half, :], in0=S[:, half, :], scalar=cdec,
                                                                      in1=bank(4 + half, 256), op0=ALU.mult, op1=ALU.add),
                              reads=["S", "ps%d" % (4 + half)], writes=["S"])
                    em.op("act", lambda e: e.copy(out=S_bf[:], in_=S[:]), reads=["S"], writes=["S_bf"])
                em.dma("sp", retp_o[h].rearrange("(hf p) e -> p hf e", p=128), S[:], reads=["S"], writes=["retp%d" % h])

                sc = slice(2 * NP, 2 * NP + NS)
                for half in range(2):
                    em.op("pe", lambda e: e.transpose(out=pst0[:NS, half * 128:(half + 1) * 128], in_=kR[:, half, sc],
                                                      identity=ident_b[:]),
                          reads=["rdstdve", "ident_b"], writes=["ps0"])
                    em.op("pe", lambda e: e.transpose(out=pst0[:NS, 256 + half * 128:256 + (half + 1) * 128],
                                                      in_=vT[b][:, half, sc], identity=ident_b[:]),
                          reads=[ld, "ident_b"], writes=["ps0"])
                kds = cst[:NS, CO["kdecs"] + h:CO["kdecs"] + h + 1]
                em.op("dve", lambda e: e.tensor_scalar(out=k_tm[0][:NS, :], in0=pst0[:NS, 0:256], scalar1=kds, scalar2=None,
                                                       op0=ALU.mult),
                      reads=["ps0", "cst"], writes=["k_tm0"])
                em.op("act", lambda e: e.copy(out=v_tm[0][:NS, :], in_=pst0[:NS, 256:512]), reads=["ps0"], writes=["v_tm0"])
                for half in range(2):
                    em.op("pe", lambda e: e.matmul(bank(1, NS)[:NS, :], lhsT=kR[:, half, sc], rhs=qR[:, half, NP:NTOK],
                                                   start=(half == 0), stop=(half == 1)),
                          reads=["rdstdve", "rdstpool"], writes=["ps1"])
                DTs = cst[:NS, CO["DTs"] + h * 64:CO["DTs"] + (h + 1) * 64]
                em.op("dve", lambda e: e.tensor_tensor(out=attb[0][:NS, :NS], in0=bank(1, NS)[:NS, :], in1=DTs, op=ALU.mult),
                      reads=["ps1", "cst"], writes=["attb0"])
                em.op("pe", lambda e: e.matmul(bank(2, 256)[:NS, :], lhsT=attb[0][:NS, :NS], rhs=v_tm[0][:NS, :],
                                               start=True, stop=True),
                      reads=["attb0", "v_tm0"], writes=["ps2"])
                em.op("act", lambda e: e.copy(out=o_st[:, :], in_=bank(2, 256)[:NS, :]), reads=["ps2"], writes=["o_st"])
                selq = cst[:, CO["selq"]:CO["selq"] + 16 * 64].rearrange("p (s i) -> p s i", s=16)
                for half in range(2):
                    em.op("pool", lambda e: e.tensor_tensor(out=qSm[:, half, :, :],
                                                            in0=qS[:, half, NP:NTOK].unsqueeze(1).broadcast_to([128, 16, NS]),
                                                            in1=selq, op=ALU.mult),
                          reads=["qS", "cst"], writes=["qSm"])
                selk = cst[:NS, CO["selk"]:CO["selk"] + 16]
                em.op("dve", lambda e: e.tensor_tensor(out=kmask[:, :, :],
                                                       in0=k_tm[0][:NS, :].unsqueeze(1).broadcast_to([NS, 16, 256]),
                                                       in1=selk.unsqueeze(2).broadcast_to([NS, 16, 256]), op=ALU.mult),
                      reads=["k_tm0", "cst"], writes=["kmask"])
                cs_ = GAM[h] ** 4
                for s_ in range(16):
                    sb3, sb2 = s_ % 3, s_ % 2
                    em.dma("sp", Ss[sb3][:], I("st_ret")[s_, h].rearrange("(hf p) e -> p hf e", p=128),
                           writes=["Ss%d" % sb3])
                    for half in range(2):
                        em.op("pe", lambda e: e.matmul(bank(3, 256)[:NS, :], lhsT=qSm[:, half, s_, :], rhs=Ss[sb3][:, half, :],
                                                       start=(s_ == 0 and half == 0), stop=(s_ == 15 and half == 1)),
                              reads=["qSm", "Ss%d" % sb3], writes=["ps3"])
                    for half in range(2):
                        em.op("pe", lambda e: e.matmul(bank(4 + half, 256), lhsT=kmask[:, s_, half * 128:(half + 1) * 128],
                                                       rhs=v_tm[0][:NS, :], start=True, stop=True),
                              reads=["kmask", "v_tm0"], writes=["ps%d" % (4 + half)])
                        em.op("dve", lambda e: e.scalar_tensor_tensor(out=Sn[sb2][:, half, :], in0=Ss[sb3][:, half, :],
                                                                      scalar=cs_, in1=bank(4 + half, 256),
                                                                      op0=ALU.mult, op1=ALU.add),
                              reads=["Ss%d" % sb3, "ps%d" % (4 + half)], writes=["Sn%d" % sb2])
                    em.dma("sp", rets_o[s_, h].rearrange("(hf p) e -> p hf e", p=128), Sn[sb2][:],
                           reads=["Sn%d" % sb2], writes=["rets_%d_%d" % (s_, h)])
                em.op("dve", lambda e: e.tensor_tensor(out=o_all[:NS, 8, :], in0=o_st[:, :], in1=bank(3, 256)[:NS, :], op=ALU.add),
                      reads=["o_st", "ps3"], writes=["o_all"])

                em.op("dve", lambda e: e.tensor_reduce(out=gst[:, 0, :], in_=o_all[:, :, :], axis=AX.X, op=ALU.add),
                      reads=["o_all"], writes=["gst"])
                for i in range(9):
                    em.op("act", lambda e: e.activation(out=o_sq[:, :], in_=o_all[:, i, :], func=AF.Square,
                                                        accum_out=gst[:, 1, i:i + 1]),
                          reads=["o_all"], writes=["o_sq", "gst"])
                em.op("dve", lambda e: e.tensor_scalar(out=gst[:, 2, :], in0=gst[:, 0, :], scalar1=1.0 / 256, scalar2=None,
                                                       op0=ALU.mult), reads=["gst"], writes=["gst"])
                em.op("dve", lambda e: e.tensor_tensor(out=gst[:, 3, :], in0=gst[:, 2, :], in1=gst[:, 2, :], op=ALU.mult),
                      reads=["gst"], writes=["gst"])
                em.op("dve", lambda e: e.scalar_tensor_tensor(out=gst[:, 3, :], in0=gst[:, 1, :], scalar=1.0 / 256,
                                                              in1=gst[:, 3, :], op0=ALU.mult, op1=ALU.subtract),
                      reads=["gst"], writes=["gst"])
                em.op("act", lambda e: e.activation(out=gst[:, 4, :], in_=gst[:, 3, :], func=AF.Sqrt, bias=eps_t[:, :], scale=1.0),
                      reads=["gst", "eps"], writes=["gst"])
                em.op("dve", lambda e: e.reciprocal(out=gst[:, 4, :], in_=gst[:, 4, :]), reads=["gst"], writes=["gst"])
                pst6 = bankbf(6)
                for i in range(9):
                    ib = i % 2
                    r = 128 if i < 8 else NS
                    qc = slice(i * 128, i * 128 + r)
                    em.op("dve", lambda e: e.tensor_scalar(out=on[ib][:r, :], in0=o_all[:r, i, :], scalar1=gst[:r, 2, i:i + 1],
                                                           scalar2=gst[:r, 4, i:i + 1], op0=ALU.subtract, op1=ALU.mult),
                          reads=["o_all", "gst"], writes=["on%d" % ib])
                    for half in range(2):
                        em.op("pe", lambda e: e.transpose(out=pst6[:, half * 128:half * 128 + r],
                                                          in_=on[ib][:r, half * 128:(half + 1) * 128], identity=ident_b[:r, :r]),
                              reads=["on%d" % ib, "ident_b"], writes=["ps6"])
                    for half in range(2):
                        gcol = cst[:, CO["gng"] + h * 2 + half:CO["gng"] + h * 2 + half + 1]
                        em.op("dve", lambda e: e.scalar_tensor_tensor(out=ogT[:, h * 2 + half, qc],
                                                                      in0=pst6[:, half * 128:half * 128 + r], scalar=gcol,
                                                                      in1=zg[b][:, half, qc], op0=ALU.mult, op1=ALU.mult),
                              reads=["ps6", "cst", ld], writes=["ogT"])
            em.barrier()

        if stop_after == "ret":
            em.finish()
            print("instructions emitted:", em.n_inst)
            return nc

        omT = sbt(br, "omT", [128, 8, NTOK], BF16)
        with contextlib.ExitStack() as ph:
            zm = sbt(ph, "zm", [128, 8, NTOK], BF16)
            for c8 in range(8):
                em.dma("sp", zm[:, c8, :], zT[(C_M + c8) * 128:(C_M + c8 + 1) * 128, :], reads=["zscr"], writes=["zm"])
            psm = sbt(ph, "psm", [128, 256], F32)
            pn = sbt(ph, "pn", [128, 256], BF16)
            pT = sbt(ph, "pT", [128, 2, 128], BF16)
            ast = sbt(ph, "ast", [128, 4], F32)
            Ksb = [sbt(ph, "Ksb", [128, 2, 1024], BF16) for _ in range(2)]
            Vsb = [sbt(ph, "Vsb", [128, 2, 1024], BF16) for _ in range(2)]
            KsT = sbt(ph, "KsT", [128, 8, NMEM], BF16)
            pst7 = bankbf(7)

            def attn(n, qf, kf, vf, of, rk):
                for half in range(2):
                    em.op("pe", lambda e: e.matmul(bank(1, 256)[:n, :], lhsT=qf(half), rhs=kf(half),
                                                   start=(half == 0), stop=(half == 1)),
                          reads=["zm"] + rk, writes=["ps1"])
                em.op("dve", lambda e: e.tensor_reduce(out=ast[:n, 0:1], in_=bank(1, 256)[:n, :], axis=AX.X, op=ALU.max),
                      reads=["ps1"], writes=["ast"])
                em.op("dve", lambda e: e.tensor_scalar(out=ast[:n, 1:2], in0=ast[:n, 0:1], scalar1=-1.0 / 16, scalar2=None,
                                                       op0=ALU.mult), reads=["ast"], writes=["ast"])
                em.op("act", lambda e: e.activation(out=psm[:n, :], in_=bank(1, 256)[:n, :], func=AF.Exp,
                                                    bias=ast[:n, 1:2], scale=1.0 / 16, accum_out=ast[:n, 2:3]),
                      reads=["ps1", "ast"], writes=["psm", "ast"])
                em.op("dve", lambda e: e.reciprocal(out=ast[:n, 3:4], in_=ast[:n, 2:3]), reads=["ast"], writes=["ast"])
                em.op("dve", lambda e: e.tensor_scalar(out=pn[:n, :], in0=psm[:n, :], scalar1=ast[:n, 3:4], scalar2=None,
                                                       op0=ALU.mult), reads=["psm", "ast"], writes=["pn"])
                for mt in range(2):
                    em.op("pe", lambda e: e.transpose(out=pst7[:, mt * 128:mt * 128 + n], in_=pn[:n, mt * 128:(mt + 1) * 128],
                                                      identity=ident_b[:n, :n]),
                          reads=["pn", "ident_b"], writes=["ps7"])
                em.op("act", lambda e: e.copy(out=pT[:, :, :n], in_=pst7[:, 0:256].rearrange("p (a c) -> p a c", a=2)[:, :, :n]),
                      reads=["ps7"], writes=["pT"])
                for dh in range(2):
                    for mt in range(2):
                        em.op("pe", lambda e: e.matmul(bank(2 + dh, 128)[:, :n], lhsT=vf(mt, dh), rhs=pT[:, mt, :n],
                                                       start=(mt == 0), stop=(mt == 1)),
                              reads=["pT"] + rk, writes=["ps%d" % (2 + dh)])
                    if dh == 0:
                        em.op("act", lambda e: e.copy(out=of(dh), in_=bank(2 + dh, 128)[:, :n]),
                              reads=["ps%d" % (2 + dh)], writes=["omT"])
                    else:
                        em.op("dve", lambda e: e.tensor_copy(out=of(dh), in_=bank(2 + dh, 128)[:, :n]),
                              reads=["ps%d" % (2 + dh)], writes=["omT"])

            for h in range(4):
                for t in range(8):
                    tc0 = t * 128
                    attn(128,
                         lambda half: zm[:, h * 2 + half, tc0:tc0 + 128],
                         lambda half: mkT[:, h * 2 + half, :],
                         lambda mt, dh: mv_tm[:, mt, h * 256 + dh * 128:h * 256 + (dh + 1) * 128],
                         lambda dh: omT[:, h * 2 + dh, tc0:tc0 + 128], ["mkT", "mv_tm"])
            for s_ in range(16):
                b = s_ % 2
                em.dma("pool", Ksb[b][:], I("c_mk")[s_].rearrange("(mt p) c -> p mt c", p=128), writes=["Ksb%d" % b])
                em.dma("pool", Vsb[b][:], I("c_mv")[s_].rearrange("(mt p) c -> p mt c", p=128), writes=["Vsb%d" % b])
                pst6 = bankbf(6)
                for mt in range(2):
                    for c8 in range(8):
                        em.op("pe", lambda e: e.transpose(out=pst6[:, c8 * 128:(c8 + 1) * 128],
                                                          in_=Ksb[b][:, mt, c8 * 128:(c8 + 1) * 128], identity=ident_b[:]),
                              reads=["Ksb%d" % b, "ident_b"], writes=["ps6"])
                    em.op("dve", lambda e: e.tensor_copy(out=KsT[:, :, mt * 128:(mt + 1) * 128],
                                                         in_=pst6.rearrange("p (c m) -> p c m", c=8)),
                          reads=["ps6"], writes=["KsT"])
                for h in range(4):
                    qsl = slice(NP + s_, NTOK, 16)
                    attn(4,
                         lambda half: zm[:, h * 2 + half, qsl],
                         lambda half: KsT[:, h * 2 + half, :],
                         lambda mt, dh: Vsb[b][:, mt, h * 256 + dh * 128:h * 256 + (dh + 1) * 128],
                         lambda dh: omT[:, h * 2 + dh, qsl], ["KsT", "Vsb%d" % b])
            em.barrier()

        with contextlib.ExitStack() as ph:
            wbf = [sbt(ph, "wbf", [128, KT, 256], BF16) for _ in range(2)]
            gt = [sbt(ph, "gt", [128, 3, NTOK], BF16) for _ in range(2)]
            m1 = sbt(ph, "m1", [128, NTOK], F32)
            m2 = sbt(ph, "m2", [128, NTOK], F32)
            mo = [sbt(ph, "mo", [128, NTOK], BF16) for _ in range(2)]
            wviews = [I("w_conv_out").rearrange("(kt p) n -> p kt n", p=128),
                      I("w_ret_out").rearrange("(kt p) n -> p kt n", p=128),
                      I("w_mem_out").rearrange("(kt p) n -> p kt n", p=128)]
            srcs = [(cnT, 8, 0), (ogT, 16, 8), (omT, 8, 24)]
            for jp in range(16):
                s = jp % 2
                for wv_, (_, nk_, k0_) in zip(wviews, srcs):
                    em.dma("pool", wbf[s][:, k0_:k0_ + nk_, :], wv_[:, :, jp * 256:(jp + 1) * 256], writes=["wbf%d" % s])
                for sub in range(2):
                    j = jp * 2 + sub
                    gb = j % 2
                    for bi in range(3):
                        em.dma("sp", gt[gb][:, bi, :], zT[(C_GATE + bi * 32 + j) * 128:(C_GATE + bi * 32 + j + 1) * 128, :],
                               reads=["zscr"], writes=["gt%d" % gb])
                    for ti, (t0, tn) in enumerate(TB):
                        for bi, (src, nk_, k0_) in enumerate(srcs):
                            pb = bi + 3 * (ti % 2)
                            for kt in range(nk_):
                                em.op("pe", lambda e: e.matmul(bank(pb, tn), lhsT=wbf[s][:, k0_ + kt, sub * 128:(sub + 1) * 128],
                                                               rhs=src[:, kt, t0:t0 + tn], start=(kt == 0), stop=(kt == nk_ - 1)),
                                      reads=["wbf%d" % s, "cnT", "ogT", "omT"], writes=["ps%d" % pb])
                        p0 = 3 * (ti % 2)
                        em.op("dve", lambda e: e.tensor_tensor(out=m1[:, t0:t0 + tn], in0=bank(p0, tn), in1=gt[gb][:, 0, t0:t0 + tn],
                                                               op=ALU.mult), reads=["ps%d" % p0, "gt%d" % gb], writes=["m1"])
                        em.op("dve", lambda e: e.tensor_tensor(out=m2[:, t0:t0 + tn], in0=bank(p0 + 1, tn), in1=gt[gb][:, 1, t0:t0 + tn],
                                                               op=ALU.mult), reads=["ps%d" % (p0 + 1), "gt%d" % gb], writes=["m2"])
                        em.op("dve", lambda e: e.tensor_tensor(out=m1[:, t0:t0 + tn], in0=m1[:, t0:t0 + tn], in1=m2[:, t0:t0 + tn],
                                                               op=ALU.add), reads=["m1", "m2"], writes=["m1"])
                        em.op("dve", lambda e: e.tensor_tensor(out=m2[:, t0:t0 + tn], in0=bank(p0 + 2, tn), in1=gt[gb][:, 2, t0:t0 + tn],
                                                               op=ALU.mult), reads=["ps%d" % (p0 + 2), "gt%d" % gb], writes=["m2"])
                        em.op("dve", lambda e: e.tensor_tensor(out=mo[gb][:, t0:t0 + tn], in0=m1[:, t0:t0 + tn], in1=m2[:, t0:t0 + tn],
                                                               op=ALU.add), reads=["m1", "m2"], writes=["mo%d" % gb])
                    em.dma("sp", mT[j * 128:(j + 1) * 128, :], mo[gb][:, :], reads=["mo%d" % gb], writes=["mscr"])
            em.barrier()
        br.close()

        with contextlib.ExitStack() as ph:
            mg = sbt(ph, "mg", [128, KT, NTOK], BF16)
            for kt in range(KT):
                em.dma("sp", mg[:, kt, :], mT[kt * 128:(kt + 1) * 128, :], reads=["mscr"], writes=["mg"])
            wbf = [sbt(ph, "wbf", [128, KT, 256], BF16) for _ in range(3)]
            xr = [sbt(ph, "xr", [128, 256], F32) for _ in range(3)]
            wv_ = I("w_out").rearrange("(kt p) n -> p kt n", p=128)
            n_ = 0
            for g in range(16):
                s = g % 3
                em.dma("pool", wbf[s][:], wv_[:, :, g * 256:(g + 1) * 256], writes=["wbf%d" % s])
                for t in range(9):
                    r = 128 if t < 8 else NS
                    pb = n_ % 8
                    xb = n_ % 3
                    n_ += 1
                    em.dma("sp", xr[xb][:r, :], I("xm")[t * 128:t * 128 + r, g * 256:(g + 1) * 256], writes=["xr%d" % xb])
                    for kt in range(KT):
                        em.op("pe", lambda e: e.matmul(bank(pb, 256)[:r, :], lhsT=mg[:, kt, t * 128:t * 128 + r],
                                                       rhs=wbf[s][:, kt, :], start=(kt == 0), stop=(kt == KT - 1)),
                              reads=["mg", "wbf%d" % s], writes=["ps%d" % pb])
                    em.op("dve", lambda e: e.tensor_tensor(out=xr[xb][:r, :], in0=xr[xb][:r, :], in1=bank(pb, 256)[:r, :], op=ALU.add),
                          reads=["xr%d" % xb, "ps%d" % pb], writes=["xr%d" % xb])
                    em.dma("sp", x1s[t * 128:t * 128 + r, g * 256:(g + 1) * 256], xr[xb][:r, :], reads=["xr%d" % xb],
                           writes=["x1scr"])
            em.barrier()

        if stop_after == "x1":
            em.finish()
            print("instructions emitted:", em.n_inst)
            return nc

        NEG = -1.0e30
        with contextlib.ExitStack() as ph:
            h2T = sbt(ph, "h2T", [128, KT, NTOK], BF16)
            with contextlib.ExitStack() as ph2:
                prep(ph2, x1s, NTOK, I("norm_ffn"), h2T, 0)
                em.barrier()
            with contextlib.ExitStack() as phq:
                qTp = sbt(phq, "qTp", [128, 16, NTOK], BF16)
                skT = sbt(phq, "skT", [128, 16, 128], BF16)
                with contextlib.ExitStack() as ph2:
                    wbf = [sbt(ph2, "wbf", [128, KT, 256], BF16) for _ in range(2)]

                    def consq(idx, j, pss, keys):
                        for (t0, tn), p_, k_ in zip(TB, pss, keys):
                            em.op("act", lambda e: e.copy(out=qTp[:, idx, t0:t0 + tn], in_=p_), reads=[k_], writes=["qTp"])
                    linear_fm(wbf, h2T, ["hT"], TB, I("w_peer_q"), KT, list(range(16)), consq)
                    skl = sbt(ph2, "skl", [128, 16, 128], BF16)
                    em.dma("pool", skl[:], I("peer_sk").rearrange("(c k) d -> k c d", k=128), writes=["skl"])
                    for g in range(2):
                        pst = bankbf(6 + g)
                        for i in range(8):
                            em.op("pe", lambda e: e.transpose(out=pst[:, i * 128:(i + 1) * 128], in_=skl[:, g * 8 + i, :],
                                                              identity=ident_b[:]),
                                  reads=["skl", "ident_b"], writes=["ps%d" % (6 + g)])
                        em.op("dve", lambda e: e.tensor_copy(out=skT[:, g * 8:(g + 1) * 8, :],
                                                             in_=pst.rearrange("p (c k) -> p c k", c=8)),
                              reads=["ps%d" % (6 + g)], writes=["skT"])
                    em.barrier()
                with contextlib.ExitStack() as ph2:
                    sc = sbt(ph2, "sc", [128, 16, 128], F32)
                    w0 = sbt(ph2, "w0", [128, 256], F32)
                    sv = sbt(ph2, "sv", [128, 16, 16], F32)
                    cand = sbt(ph2, "cand", [128, 256], F32)
                    tv = sbt(ph2, "tv", [128, 8, 16], F32)
                    ex = sbt(ph2, "ex", [128, 8, 16], F32)
                    pstat = sbt(ph2, "pstat", [128, 4, 8], F32)
                    Lt = [sbt(ph2, "Lt", [128, 16, 128], F32) for _ in range(2)]
                    Et = [sbt(ph2, "Et", [128, 2048], BF16) for _ in range(2)]
                    Gt = [sbt(ph2, "Gt", [128, 2048], BF16)] * 2
                    Wt = sbt(ph2, "Wt", [128, 16384], BF16)
                    for t in range(9):
                        r = 128 if t < 8 else NS
                        tcs = slice(t * 128, t * 128 + r)
                        for c in range(16):
                            em.op("pe", lambda e: e.matmul(bank(c // 4, 128, (c % 4) * 128)[:r, :], lhsT=qTp[:, c, tcs],
                                                           rhs=skT[:, c, :], start=True, stop=True),
                                  reads=["qTp", "skT"], writes=["ps%d" % (c // 4)])
                        for g in range(4):
                            em.op("act", lambda e: e.copy(out=sc[:r, g * 4:(g + 1) * 4, :],
                                                          in_=bank(g)[:r, :].rearrange("p (c k) -> p c k", c=4)),
                                  reads=["ps%d" % g], writes=["sc"])
                        for c in range(16):
                            em.op("dve", lambda e: e.max(out=sv[:r, c, 0:8], in_=sc[:r, c, :]), reads=["sc"], writes=["sv"])
                            em.op("dve", lambda e: e.match_replace(out=w0[:r, 0:128], in_to_replace=sv[:r, c, 0:8],
                                                                   in_values=sc[:r, c, :], imm_value=NEG),
                                  reads=["sc", "sv"], writes=["w0"])
                            em.op("dve", lambda e: e.max(out=sv[:r, c, 8:16], in_=w0[:r, 0:128]), reads=["w0"], writes=["sv"])
                        for p in range(8):
                            em.op("dve", lambda e: e.tensor_tensor(
                                out=cand[:r, :].rearrange("p (a b) -> p a b", a=16),
                                in0=sv[:r, 2 * p, :].unsqueeze(2).broadcast_to([r, 16, 16]),
                                in1=sv[:r, 2 * p + 1, :].unsqueeze(1).broadcast_to([r, 16, 16]), op=ALU.add),
                                reads=["sv"], writes=["cand"])
                            em.op("dve", lambda e: e.max(out=tv[:r, p, 0:8], in_=cand[:r, :]), reads=["cand"], writes=["tv"])
                            em.op("dve", lambda e: e.match_replace(out=w0[:r, :], in_to_replace=tv[:r, p, 0:8],
                                                                   in_values=cand[:r, :], imm_value=NEG),
                                  reads=["cand", "tv"], writes=["w0"])
                            em.op("dve", lambda e: e.max(out=tv[:r, p, 8:16], in_=w0[:r, :]), reads=["w0"], writes=["tv"])
                        em.op("dve", lambda e: e.tensor_scalar(out=pstat[:r, 0, :], in0=tv[:r, :, 0], scalar1=-1.0, scalar2=None,
                                                               op0=ALU.mult), reads=["tv"], writes=["pstat"])
                        em.op("dve", lambda e: e.tensor_copy(out=pstat[:r, 3, :], in_=tv[:r, :, 15]), reads=["tv"], writes=["pstat"])
                        em.op("dve", lambda e: e.tensor_tensor(out=ex[:r], in0=tv[:r],
                                                               in1=pstat[:r, 0, :].unsqueeze(2).broadcast_to([r, 8, 16]), op=ALU.add),
                              reads=["tv", "pstat"], writes=["ex"])
                        em.op("act", lambda e: e.activation(out=ex[:r], in_=ex[:r], func=AF.Exp), reads=["ex"], writes=["ex"])
                        em.op("dve", lambda e: e.tensor_reduce(out=pstat[:r, 1, :], in_=ex[:r], axis=AX.X, op=ALU.add),
                              reads=["ex"], writes=["pstat"])
                        em.op("act", lambda e: e.activation(out=pstat[:r, 1, :], in_=pstat[:r, 1, :], func=AF.Ln),
                              reads=["pstat"], writes=["pstat"])
                        em.op("dve", lambda e: e.tensor_tensor(out=pstat[:r, 2, :], in0=pstat[:r, 0, :], in1=pstat[:r, 1, :],
                                                               op=ALU.subtract), reads=["pstat"], writes=["pstat"])
                        n_ = 0
                        for p in range(8):
                            for ib in range(8):
                                lb = n_ % 2
                                n_ += 1
                                em.op("dve", lambda e: e.tensor_tensor(
                                    out=Lt[lb][:r],
                                    in0=sc[:r, 2 * p, ib * 16:(ib + 1) * 16].unsqueeze(2).broadcast_to([r, 16, 128]),
                                    in1=sc[:r, 2 * p + 1, :].unsqueeze(1).broadcast_to([r, 16, 128]), op=ALU.add),
                                    reads=["sc"], writes=["Lt%d" % lb])
                                em.op("act", lambda e: e.activation(out=Et[lb][:r, :], in_=Lt[lb][:r].rearrange("p a b -> p (a b)"),
                                                                    func=AF.Exp, bias=pstat[:r, 2, p:p + 1], scale=1.0),
                                      reads=["Lt%d" % lb, "pstat"], writes=["Et%d" % lb])
                                wsl = Wt[:r, ib * 2048:(ib + 1) * 2048]
                                if p == 0:
                                    em.op("dve", lambda e: e.scalar_tensor_tensor(
                                        out=wsl, in0=Lt[lb][:r].rearrange("p a b -> p (a b)"), scalar=pstat[:r, 3, p:p + 1],
                                        in1=Et[lb][:r, :], op0=ALU.is_ge, op1=ALU.mult),
                                        reads=["Lt%d" % lb, "Et%d" % lb, "pstat"], writes=["Wt"])
                                else:
                                    em.op("dve", lambda e: e.scalar_tensor_tensor(
                                        out=Gt[lb][:r, :], in0=Lt[lb][:r].rearrange("p a b -> p (a b)"), scalar=pstat[:r, 3, p:p + 1],
                                        in1=Et[lb][:r, :], op0=ALU.is_ge, op1=ALU.mult),
                                        reads=["Lt%d" % lb, "Et%d" % lb, "pstat"], writes=["Gt"])
                                    em.op("pool", lambda e: e.tensor_tensor(out=wsl, in0=wsl, in1=Gt[lb][:r, :], op=ALU.add),
                                          reads=["Gt", "Wt"], writes=["Wt"])
                        em.dma("sp", Wd[t * 128:t * 128 + r, :], Wt[:r, :], reads=["Wt"], writes=["Wd"])
                    em.barrier()
            with contextlib.ExitStack() as ph2:
                ub = [sbt(ph2, "ub", [128, 2, D], BF16) for _ in range(2)]
                uT = [sbt(ph2, "uT", [128, KT, 512], BF16) for _ in range(2)]
                actt = [sbt(ph2, "actt", [128, 512], BF16) for _ in range(2)]
                Wl = [sbt(ph2, "Wl", [128, 512], BF16) for _ in range(2)]
                Tt = [sbt(ph2, "Tt", [128, 512], BF16) for _ in range(2)]
                TTt = [sbt(ph2, "TTt", [128, 4, 128], BF16) for _ in range(2)]
                uview = I("peer_u").rearrange("(n p) d -> p n d", p=128)
                nA = 0
                for eb in range(32):
                    ui = eb % 2
                    for half in range(2):
                        hb = (eb * 2 + half) % 2
                        em.dma("pool", ub[hb][:], uview[:, eb * 4 + half * 2:eb * 4 + half * 2 + 2, :], writes=["ub%d" % hb])
                        for et in range(2):
                            ecol = (half * 2 + et) * 128
                            for g in range(4):
                                pb = 6 + (g % 2)
                                pst = bankbf(pb)
                                for i in range(8):
                                    kt = g * 8 + i
                                    em.op("pe", lambda e: e.transpose(out=pst[:, i * 128:(i + 1) * 128],
                                                                      in_=ub[hb][:, et, kt * 128:(kt + 1) * 128], identity=ident_b[:]),
                                          reads=["ub%d" % hb, "ident_b"], writes=["ps%d" % pb])
                                dst = uT[ui][:, g * 8:(g + 1) * 8, ecol:ecol + 128]
                                src = pst.rearrange("p (i c) -> p i c", i=8)
                                if g % 2 == 0:
                                    em.op("act", lambda e: e.copy(out=dst, in_=src), reads=["ps%d" % pb], writes=["uT%d" % ui])
                                else:
                                    em.op("dve", lambda e: e.tensor_copy(out=dst, in_=src), reads=["ps%d" % pb], writes=["uT%d" % ui])
                    for t in range(9):
                        r = 128 if t < 8 else NS
                        tcs = slice(t * 128, t * 128 + r)
                        pb = nA % 6
                        ab = nA % 2
                        nA += 1
                        em.dma("sp", Wl[ab][:r, :], Wd[t * 128:t * 128 + r, eb * 512:(eb + 1) * 512], reads=["Wd"], writes=["Wl%d" % ab])
                        for kt in range(KT):
                            em.op("pe", lambda e: e.matmul(bank(pb)[:r, :], lhsT=h2T[:, kt, tcs], rhs=uT[ui][:, kt, :],
                                                           start=(kt == 0), stop=(kt == KT - 1)),
                                  reads=["hT", "uT%d" % ui], writes=["ps%d" % pb])
                        em.op("act", lambda e: e.activation(out=actt[ab][:r, :], in_=bank(pb)[:r, :], func=AF.Gelu_apprx_tanh),
                              reads=["ps%d" % pb], writes=["actt%d" % ab])
                        em.op("dve", lambda e: e.tensor_tensor(out=Tt[ab][:r, :], in0=actt[ab][:r, :], in1=Wl[ab][:r, :], op=ALU.mult),
                              reads=["actt%d" % ab, "Wl%d" % ab], writes=["Tt%d" % ab])
                        pst = bankbf(7 if ab else 6)
                        pk = "ps%d" % (7 if ab else 6)
                        for et in range(4):
                            em.op("pe", lambda e: e.transpose(out=pst[:, et * 128:et * 128 + r], in_=Tt[ab][:r, et * 128:(et + 1) * 128],
                                                              identity=ident_b[:r, :r]),
                                  reads=["Tt%d" % ab, "ident_b"], writes=[pk])
                        em.op("act", lambda e: e.copy(out=TTt[ab][:, :, :r], in_=pst[:, 0:512].rearrange("p (a c) -> p a c", a=4)[:, :, :r]),
                              reads=[pk], writes=["TTt%d" % ab])
                        em.dma("sp", TTd[eb * 512:(eb + 1) * 512, tcs].rearrange("(a p) n -> p a n", p=128), TTt[ab][:, :, :r],
                               reads=["TTt%d" % ab], writes=["TTd"])
                em.barrier()

        with contextlib.ExitStack() as ph:
            vb = [sbt(ph, "vb", [128, 8, 512], BF16) for _ in range(3)]
            ttb = [sbt(ph, "ttb", [128, 8, 640], BF16) for _ in range(3)]
            x1t = [sbt(ph, "x1t", [128, 512], F32) for _ in range(3)]
            vview = I("peer_v").rearrange("(n p) d -> p n d", p=128)
            tview = TTd.rearrange("(n p) t -> p n t", p=128)
            nB = 0
            nx = 0
            for dg in range(8):
                for tg, (g0, tiles) in enumerate(((0, [0, 1, 2, 3, 4]), (640, [5, 6, 7, 8]))):
                    gn = 640 if tg == 0 else NTOK - 640
                    for ec in range(16):
                        bb = nB % 3
                        nB += 1
                        em.dma("pool", vb[bb][:], vview[:, ec * 8:(ec + 1) * 8, dg * 512:(dg + 1) * 512], writes=["vb%d" % bb])
                        em.dma("sp", ttb[bb][:, :, :gn], tview[:, ec * 8:(ec + 1) * 8, g0:g0 + gn], reads=["TTd"], writes=["ttb%d" % bb])
                        for et in range(8):
                            for li, t in enumerate(tiles):
                                r = 128 if t < 8 else NS
                                lc = t * 128 - g0
                                em.op("pe", lambda e: e.matmul(bank(li)[:r, :], lhsT=ttb[bb][:, et, lc:lc + r], rhs=vb[bb][:, et, :],
                                                               start=(ec == 0 and et == 0), stop=(ec == 15 and et == 7)),
                                      reads=["vb%d" % bb, "ttb%d" % bb], writes=["ps%d" % li])
                    for li, t in enumerate(tiles):
                        r = 128 if t < 8 else NS
                        xb = nx % 3
                        nx += 1
                        em.dma("sp", x1t[xb][:r, :], x1s[t * 128:t * 128 + r, dg * 512:(dg + 1) * 512], reads=["x1scr"], writes=["x1t%d" % xb])
                        em.op("dve", lambda e: e.tensor_tensor(out=x1t[xb][:r, :], in0=x1t[xb][:r, :], in1=bank(li)[:r, :], op=ALU.add),
                              reads=["x1t%d" % xb, "ps%d" % li], writes=["x1t%d" % xb])
                        em.dma("sp", x2s[t * 128:t * 128 + r, dg * 512:(dg + 1) * 512], x1t[xb][:r, :], reads=["x1t%d" % xb], writes=["x2scr"])
            em.barrier()
        with contextlib.ExitStack() as ph:
            gbc = sbt(ph, "gbcf", [128, D], F32)
            xt = [sbt(ph, "xtf", [128, D], F32) for _ in range(2)]
            yt = [sbt(ph, "ytf", [128, D], F32) for _ in range(2)]
            junk = sbt(ph, "junkf", [128, D], BF16)
            stat = [sbt(ph, "statf", [128, 4], F32) for _ in range(2)]
            em.dma("sp", gbc[:], I("norm_final").broadcast_to([128, D]), writes=["gbc"])
            for t in range(9):
                r = 128 if t < 8 else NS
                b = t % 2
                em.dma("sp", xt[b][:r, :], x2s[t * 128:t * 128 + r, :], reads=["x2scr"], writes=["xt%d" % b])
                em.op("act", lambda e: e.activation(out=junk[:r, :], in_=xt[b][:r, :], func=AF.Square, accum_out=stat[b][:r, 0:1]),
                      reads=["xt%d" % b], writes=["junk", "st%d" % b])
                em.op("act", lambda e: e.activation(out=stat[b][:r, 1:2], in_=stat[b][:r, 0:1], func=AF.Sqrt, bias=eps_t[:r, :],
                                                    scale=1.0 / D), reads=["st%d" % b, "eps"], writes=["st%d" % b])
                em.op("dve", lambda e: e.reciprocal(out=stat[b][:r, 2:3], in_=stat[b][:r, 1:2]), reads=["st%d" % b], writes=["st%d" % b])
                em.op("dve", lambda e: e.scalar_tensor_tensor(out=yt[b][:r, :], in0=xt[b][:r, :], scalar=stat[b][:r, 2:3],
                                                              in1=gbc[:r, :], op0=ALU.mult, op1=ALU.mult),
                      reads=["xt%d" % b, "st%d" % b, "gbc"], writes=["yt%d" % b])
                em.dma("sp", y_o[t * 128:t * 128 + r, :], yt[b][:r, :], reads=["yt%d" % b], writes=["y_%d" % t])

        em.finish()
        print("instructions emitted:", em.n_inst)
    return nc


def _consts():
    c = np.zeros((128, CW), np.float32)
    c[:, CO["ident"]:CO["ident"] + 128] = np.eye(128, dtype=np.float32)
    i = np.arange(128)
    for h in range(8):
        g = GAM[h]
        dd = i[None, :] - i[:, None]
        c[:, CO["DT"] + h * 128: CO["DT"] + (h + 1) * 128] = np.where(dd >= 0, g ** np.maximum(dd, 0), 0.0) / 16.0
        c[:, CO["qdec"] + h * 128: CO["qdec"] + (h + 1) * 128] = (g ** (i + 1.0))[None, :] / 16.0
        c[:, CO["kdec"] + h] = g ** (127.0 - i)
        idx = np.arange(64)
        t_i, s_i = idx // 16, idx % 16
        dts = np.where((s_i[None, :] == s_i[:, None]) & (t_i[None, :] >= t_i[:, None]),
                       g ** np.maximum(t_i[None, :] - t_i[:, None], 0).astype(np.float64), 0.0) / 16.0
        c[:64, CO["DTs"] + h * 64: CO["DTs"] + (h + 1) * 64] = dts
        c[:, CO["qdecs"] + h * 64: CO["qdecs"] + (h + 1) * 64] = (g ** (t_i + 1.0))[None, :] / 16.0
        c[:64, CO["kdecs"] + h] = g ** (3.0 - t_i)
    idx = np.arange(64)
    for s in range(16):
        c[:, CO["selq"] + s * 64: CO["selq"] + (s + 1) * 64] = (idx % 16 == s).astype(np.float32)[None, :]
        c[:64, CO["selk"] + s] = (idx % 16 == s).astype(np.float32)
    return c


def _rot(pos0):
    half = 128
    inv = (np.float32(10000.0) ** (-(np.arange(half, dtype=np.float32)) / np.float32(half))).astype(np.float32)
    pos = np.concatenate([np.arange(NP, dtype=np.float32) + np.float32(pos0 - NP),
                          np.arange(NP, dtype=np.float32) + np.float32(pos0),
                          np.float32(16384.0) + (np.arange(64) // 16).astype(np.float32)]).astype(np.float32)
    ang = (pos[None, :] * inv[:, None]).astype(np.float32).astype(np.float64)
    r = np.zeros((128, 2, 2112), np.float32)
    r[:, 0, :] = np.cos(ang)
    r[:, 1, :] = np.sin(ang)
    return r


def make_in_maps(x_prompt, x_sample, mem_prompt, state_conv, state_ret, cache_mem_k, cache_mem_v,
                 norm_mix, norm_mem, w_in, conv_w, conv_b, conv_ln_g, conv_ln_b, w_conv_out,
                 ret_gn_g, w_ret_out, w_mem_k, w_mem_v, w_mem_out, w_out, norm_ffn,
                 w_peer_q, peer_subkeys, peer_u, peer_v, norm_final):
    f = lambda a: np.ascontiguousarray(np.asarray(a, dtype=np.float32))
    cst = _consts()
    cst[:, CO["convw"]:CO["convw"] + 8 * 31] = f(conv_w[0]).reshape(31, 8, 128).transpose(2, 1, 0).reshape(128, 8 * 31)
    cst[:, CO["convb"]:CO["convb"] + 8] = f(conv_b[0]).reshape(8, 128).T
    cst[:, CO["lng"]:CO["lng"] + 8] = f(conv_ln_g[0]).reshape(8, 128).T
    cst[:, CO["lnb"]:CO["lnb"] + 8] = f(conv_ln_b[0]).reshape(8, 128).T
    cst[:, CO["gng"]:CO["gng"] + 16] = f(ret_gn_g[0]).reshape(16, 128).T
    shared = {
        "norm_mix": f(norm_mix[0:1]), "norm_mem": f(norm_mem[0:1]), "norm_ffn": f(norm_ffn[0:1]),
        "norm_final": f(norm_final).reshape(1, D),
        "w_in": f(w_in[0]), "w_conv_out": f(w_conv_out[0]), "w_ret_out": f(w_ret_out[0]),
        "w_mem_k": f(w_mem_k[0]), "w_mem_v": f(w_mem_v[0]), "w_mem_out": f(w_mem_out[0]),
        "w_out": f(w_out[0]), "w_peer_q": f(w_peer_q[0]),
        "peer_sk": f(peer_subkeys[0]).reshape(16 * 128, 128),
        "peer_u": f(peer_u[0]), "peer_v": f(peer_v[0]), "cst": cst,
    }
    xs = f(x_sample)
    in_maps = []
    for c in range(NCORES):
        b, hf = c // 2, c % 2
        xmain = np.empty((NTOK, D), np.float32)
        xmain[:NP] = x_prompt[b, hf * NP:(hf + 1) * NP]
        xmain[NP:] = xs[c * 16:(c + 1) * 16].transpose(1, 0, 2).reshape(NS, D)
        xp = np.zeros((NP, D), np.float32)
        if hf == 1:
            xp[:] = x_prompt[b, 0:NP]
        m = dict(shared)
        m.update({
            "xm": xmain, "xpre": xp, "mem": f(mem_prompt[b]),
            "st_conv": f(state_conv[0, c * 16:(c + 1) * 16]),
            "st_ret": f(state_ret[0, c * 16:(c + 1) * 16]),
            "c_mk": f(cache_mem_k[0, c * 16:(c + 1) * 16]).reshape(16, 256, 1024),
            "c_mv": f(cache_mem_v[0, c * 16:(c + 1) * 16]).reshape(16, 256, 1024),
            "rot": _rot(hf * NP),
        })
        in_maps.append(m)
    return in_maps


def assemble(R):
    y_prompt = np.zeros((4, 2048, D), np.float32)
    y_sample = np.zeros((128, 4, D), np.float32)
    conv_p = np.zeros((1, 4, 30, 1024), np.float32)
    ret_p = np.zeros((1, 4, 8, 256, 256), np.float32)
    memk_p = np.zeros((1, 4, 256, 4, 256), np.float32)
    memv_p = np.zeros((1, 4, 256, 4, 256), np.float32)
    conv_s = np.zeros((1, 128, 30, 1024), np.float32)
    ret_s = np.zeros((1, 128, 8, 256, 256), np.float32)
    for c in range(NCORES):
        b, hf = c // 2, c % 2
        r = R[c]
        y_prompt[b, hf * NP:(hf + 1) * NP] = r["y_o"][:NP]
        y_sample[c * 16:(c + 1) * 16] = r["y_o"][NP:].reshape(4, 16, D).transpose(1, 0, 2)
        if hf == 0:
            memk_p[0, b] = r["memk_o"].reshape(256, 4, 256)
            memv_p[0, b] = r["memv_o"].reshape(256, 4, 256)
        else:
            conv_p[0, b] = r["convp_o"][2:32]
            ret_p[0, b] = r["retp_o"]
        conv_s[0, c * 16:(c + 1) * 16] = r["convs_o"]
        ret_s[0, c * 16:(c + 1) * 16] = r["rets_o"]
    return (y_prompt, y_sample, conv_p, ret_p, memk_p, memv_p, conv_s, ret_s)


def kernel(**inputs):
    nc = build()
    in_maps = make_in_maps(**inputs)
    in_maps = [{k: v for k, v in m.items() if k in nc.used_inputs} for m in in_maps]
    res = run_bass_kernel_spmd(nc, in_maps, core_ids=list(range(NCORES)))
    return assemble(res.results)
```
